# Optimizing a Trainium2 kernel written in Bass

```python
import jax, jax.numpy as jnp
from jax import lax
import numpy as np

D_MODEL = 2048
BATCH = 4
SEQ = 8192
DEPTH = 1

LRU_WIDTH = 1024
LRU_BLOCKS = 8
LRU_BLOCK_DIM = LRU_WIDTH // LRU_BLOCKS
CONV_WIDTH = 4
LRU_C = 8.0
ATTN_HEADS = 8
HEAD_DIM = 128
ATTN_WIDTH = ATTN_HEADS * HEAD_DIM
MOBA_BLOCK = 256
MOBA_TOPK = 3
QUERY_CHUNK = 32
N_GROUPS = 4
EXPERTS_PER_GROUP = 8
N_EXPERTS = N_GROUPS * EXPERTS_PER_GROUP
TOPK_IN_GROUP = 2
EXPERT_HIDDEN = 512
RMS_EPS = 1e-6

IN_WIDTH = 2 * LRU_WIDTH + 3 * ATTN_WIDTH + 2 * D_MODEL
SPLIT_POINTS = (LRU_WIDTH, 2 * LRU_WIDTH, 2 * LRU_WIDTH + ATTN_WIDTH, 2 * LRU_WIDTH + 2 * ATTN_WIDTH, 2 * LRU_WIDTH + 3 * ATTN_WIDTH, 2 * LRU_WIDTH + 3 * ATTN_WIDTH + D_MODEL)

kernel_name = "hybrid_rglru_moba_hmoe_block"


def rms_norm(x, g):
    xf = x.astype(jnp.float32)
    y = xf * lax.rsqrt(jnp.mean(xf * xf, axis=-1, keepdims=True) + RMS_EPS)
    return (y * g.astype(jnp.float32)).astype(x.dtype)


def causal_conv(x, w, b):
    S = x.shape[1]
    xp = jnp.pad(x, ((0, 0), (CONV_WIDTH - 1, 0), (0, 0)))
    y = b
    for k in range(CONV_WIDTH):
        y = y + xp[:, k:k + S] * w[k]
    return y


def _lin_rec_combine(c1, c2):
    a1, b1 = c1
    a2, b2 = c2
    return a1 * a2, a2 * b1 + b2


def rglru(x, wa, ba, wx, bx, lam):
    B, S, W = x.shape
    xb = x.reshape(B, S, LRU_BLOCKS, LRU_BLOCK_DIM)
    r = jax.nn.sigmoid(jnp.einsum('bsnc,ncd->bsnd', xb, wa).reshape(B, S, W) + ba)
    i = jax.nn.sigmoid(jnp.einsum('bsnc,ncd->bsnd', xb, wx).reshape(B, S, W) + bx)
    log_a = LRU_C * r.astype(jnp.float32) * jax.nn.log_sigmoid(lam.astype(jnp.float32))
    a = jnp.exp(log_a)
    b_in = jnp.sqrt(-jnp.expm1(2.0 * log_a)) * (i * x).astype(jnp.float32)
    _, h = lax.associative_scan(_lin_rec_combine, (a, b_in), axis=1)
    return h.astype(x.dtype)


def alibi_slopes(n_heads):
    return 2.0 ** (-8.0 * jnp.arange(1, n_heads + 1, dtype=jnp.float32) / n_heads)


def moba_attention(q, k, v):
    B, S, H, dh = q.shape
    nb = -(-S // MOBA_BLOCK)
    s_pad = nb * MOBA_BLOCK
    pad = ((0, 0), (0, s_pad - S), (0, 0), (0, 0))
    q, k, v = [jnp.pad(t, pad).transpose(0, 2, 1, 3) for t in (q, k, v)]
    k_blocks = k.reshape(B, H, nb, MOBA_BLOCK, dh)
    v_blocks = v.reshape(B, H, nb, MOBA_BLOCK, dh)
    k_mean = jnp.mean(k_blocks.astype(jnp.float32), axis=3)
    n_sel = min(MOBA_TOPK, nb)
    slopes = alibi_slopes(H)
    scale = HEAD_DIM ** -0.5
    b_idx = jnp.arange(B)[:, None, None, None]
    h_idx = jnp.arange(H)[None, :, None, None]
    blk_pos = jnp.arange(MOBA_BLOCK)
    f32 = jnp.float32

    def chunk(c):
        start = c * QUERY_CHUNK
        j = start // MOBA_BLOCK
        q_c = lax.dynamic_slice_in_dim(q, start, QUERY_CHUNK, axis=2)
        t = start + jnp.arange(QUERY_CHUNK)
        gate = jnp.einsum('bhqd,bhnd->bhqn', q_c.astype(f32), k_mean)
        gate = jnp.where(jnp.arange(nb) < j, gate, -jnp.inf)
        _, idx = lax.top_k(gate, n_sel)
        valid = idx < j
        k_g = k_blocks[b_idx, h_idx, idx]
        v_g = v_blocks[b_idx, h_idx, idx]
        s_sel = jnp.einsum('bhqd,bhqnkd->bhqnk', q_c, k_g, preferred_element_type=f32) * scale
        dist_sel = (t[:, None, None] - (idx[..., None] * MOBA_BLOCK + blk_pos)).astype(f32)
        s_sel = s_sel - slopes[:, None, None, None] * dist_sel
        s_sel = jnp.where(valid[..., None], s_sel, -jnp.inf)
        k_own = lax.dynamic_index_in_dim(k_blocks, j, axis=2, keepdims=False)
        v_own = lax.dynamic_index_in_dim(v_blocks, j, axis=2, keepdims=False)
        s_own = jnp.einsum('bhqd,bhkd->bhqk', q_c, k_own, preferred_element_type=f32) * scale
        dist_own = (t[:, None] - (j * MOBA_BLOCK + blk_pos)).astype(f32)
        s_own = jnp.where(dist_own >= 0, s_own - slopes[:, None, None] * dist_own, -jnp.inf)
        scores = jnp.concatenate([s_sel.reshape(B, H, QUERY_CHUNK, n_sel * MOBA_BLOCK), s_own], axis=-1)
        p = jax.nn.softmax(scores, axis=-1)
        p_sel = p[..., :n_sel * MOBA_BLOCK].reshape(B, H, QUERY_CHUNK, n_sel, MOBA_BLOCK).astype(v.dtype)
        p_own = p[..., n_sel * MOBA_BLOCK:].astype(v.dtype)
        out = (jnp.einsum('bhqnk,bhqnkd->bhqd', p_sel, v_g, preferred_element_type=f32)
               + jnp.einsum('bhqk,bhkd->bhqd', p_own, v_own, preferred_element_type=f32))
        return out.astype(q.dtype)

    out = lax.map(chunk, jnp.arange(s_pad // QUERY_CHUNK))
    out = out.transpose(1, 0, 3, 2, 4).reshape(B, s_pad, H * dh)
    return out[:, :S]


def hier_moe(h, wg_r, bg_r, we_r, be_r, w_gate, w_up, w_down):
    B, S, D = h.shape
    tok = h.reshape(B * S, D)
    f32 = jnp.float32
    grp_p = jax.nn.softmax((tok @ wg_r).astype(f32) + bg_r.astype(f32), axis=-1)
    grp_w, grp_i = lax.top_k(grp_p, 1)
    exp_logits = ((tok @ we_r).astype(f32) + be_r.astype(f32)).reshape(-1, N_GROUPS, EXPERTS_PER_GROUP)
    in_grp = jnp.take_along_axis(exp_logits, grp_i[:, :, None], axis=1)[:, 0]
    ew, ei = lax.top_k(jax.nn.softmax(in_grp, axis=-1), TOPK_IN_GROUP)
    ew = ew / jnp.sum(ew, axis=-1, keepdims=True)
    weights = grp_w * ew
    global_i = grp_i * EXPERTS_PER_GROUP + ei
    combine = jnp.sum(jax.nn.one_hot(global_i, N_EXPERTS, dtype=f32) * weights[..., None], axis=1)

    def body(acc, xs):
        wg, wu, wd, c = xs
        y = (jax.nn.silu(tok @ wg) * (tok @ wu)) @ wd
        return acc + c[:, None] * y.astype(f32), None

    acc, _ = lax.scan(body, jnp.zeros((B * S, D), f32), (w_gate, w_up, w_down, combine.T))
    return acc.astype(h.dtype).reshape(B, S, D)


def setup_inputs(seed: int = 0) -> dict:
    key = jax.random.key(seed)
    ks = jax.random.split(key, 24)
    L, D = DEPTH, D_MODEL
    nrm = lambda k, shape, s: jax.random.normal(k, shape, jnp.float32) * s
    u = jax.random.uniform(ks[9], (L, LRU_WIDTH), jnp.float32, 0.9, 0.999)
    a0 = u ** (1.0 / LRU_C)
    lam = jnp.log(a0) - jnp.log1p(-a0)
    return {
        "x": nrm(ks[0], (BATCH, SEQ, D), 1.0),
        "norm_attn_g": 1.0 + nrm(ks[1], (L, D), 0.02),
        "w_in": nrm(ks[2], (L, D, IN_WIDTH), D ** -0.5),
        "conv_w": nrm(ks[3], (L, CONV_WIDTH, LRU_WIDTH), CONV_WIDTH ** -0.5),
        "conv_b": nrm(ks[4], (L, LRU_WIDTH), 0.01),
        "lru_wa": nrm(ks[5], (L, LRU_BLOCKS, LRU_BLOCK_DIM, LRU_BLOCK_DIM), LRU_BLOCK_DIM ** -0.5),
        "lru_ba": nrm(ks[6], (L, LRU_WIDTH), 0.01),
        "lru_wx": nrm(ks[7], (L, LRU_BLOCKS, LRU_BLOCK_DIM, LRU_BLOCK_DIM), LRU_BLOCK_DIM ** -0.5),
        "lru_bx": nrm(ks[8], (L, LRU_WIDTH), 0.01),
        "lru_lambda": lam,
        "proj_rec": nrm(ks[10], (L, LRU_WIDTH, D), LRU_WIDTH ** -0.5),
        "proj_attn": nrm(ks[11], (L, ATTN_WIDTH, D), ATTN_WIDTH ** -0.5),
        "w_out": nrm(ks[12], (L, D, D), D ** -0.5),
        "norm_ffn_g": 1.0 + nrm(ks[13], (L, D), 0.02),
        "router_group_w": nrm(ks[14], (L, D, N_GROUPS), D ** -0.5),
        "router_group_b": nrm(ks[15], (L, N_GROUPS), 0.01),
        "router_expert_w": nrm(ks[16], (L, D, N_EXPERTS), D ** -0.5),
        "router_expert_b": nrm(ks[17], (L, N_EXPERTS), 0.01),
        "expert_w_gate": nrm(ks[18], (L, N_EXPERTS, D, EXPERT_HIDDEN), D ** -0.5),
        "expert_w_up": nrm(ks[19], (L, N_EXPERTS, D, EXPERT_HIDDEN), D ** -0.5),
        "expert_w_down": nrm(ks[20], (L, N_EXPERTS, EXPERT_HIDDEN, D), EXPERT_HIDDEN ** -0.5),
        "norm_final_g": 1.0 + nrm(ks[21], (D,), 0.02),
    }


def reference(x, norm_attn_g, w_in, conv_w, conv_b, lru_wa, lru_ba, lru_wx, lru_bx, lru_lambda, proj_rec, proj_attn, w_out, norm_ffn_g, router_group_w, router_group_b, router_expert_w, router_expert_b, expert_w_gate, expert_w_up, expert_w_down, norm_final_g):
    B, S, _ = x.shape
    for l in range(DEPTH):
        h = rms_norm(x, norm_attn_g[l])
        u = h @ w_in[l]
        x_rec, g_rec, q, k, v, gate_rec, gate_attn = jnp.split(u, SPLIT_POINTS, axis=-1)
        y_rec = rglru(causal_conv(x_rec, conv_w[l], conv_b[l]), lru_wa[l], lru_ba[l], lru_wx[l], lru_bx[l], lru_lambda[l])
        y_rec = y_rec * jax.nn.gelu(g_rec, approximate=True)
        y_attn = moba_attention(q.reshape(B, S, ATTN_HEADS, HEAD_DIM), k.reshape(B, S, ATTN_HEADS, HEAD_DIM), v.reshape(B, S, ATTN_HEADS, HEAD_DIM))
        merged = jax.nn.sigmoid(gate_rec) * (y_rec @ proj_rec[l]) + jax.nn.sigmoid(gate_attn) * (y_attn @ proj_attn[l])
        x = x + merged @ w_out[l]
        x = x + hier_moe(rms_norm(x, norm_ffn_g[l]), router_group_w[l], router_group_b[l], router_expert_w[l], router_expert_b[l], expert_w_gate[l], expert_w_up[l], expert_w_down[l])
    return rms_norm(x, norm_final_g)
```

```python
import contextlib
import numpy as np
import concourse.bass as bass
import concourse.mybir as mybir
from concourse.bass_utils import run_bass_kernel_spmd

F32 = mybir.dt.float32
BF16 = mybir.dt.bfloat16
I32 = mybir.dt.int32
AF = mybir.ActivationFunctionType
ALU = mybir.AluOpType
AX = mybir.AxisListType

ENGS = ["pe", "act", "dve", "pool", "sp"]


class Sem:
    def __init__(self, handle, key):
        self.handle = handle
        self.key = key
        self.count = 0


class Buf:
    def __init__(self, ap, name=""):
        self.ap = ap
        self.name = name
        self.w = None
        self.r = []
        self.dsem = None

    def __getitem__(self, k):
        return self.ap[k]


class _Rec:
    def __init__(self):
        self.call = None

    def __getattr__(self, name):
        def f(*a, **k):
            self.call = (name, a, k)
            return self
        return f


class Prog:
    def __init__(self, nc):
        self.nc = nc
        self.q = {e: [] for e in ENGS}
        self.cnt = {e: 0 for e in ENGS}
        self.sem = {}
        self.waited = {e: {} for e in ENGS}
        self.stack = contextlib.ExitStack()
        self.nsem = 0
        self.free_dma_sems = {"hw": [], "sw": []}
        self.dma_toks = {}
        self.arena_off = 0
        self.arena = None
        self.psum = None
        for e in ("pe", "act", "dve", "pool"):
            self.sem[e] = self.new_sem(e)

    def new_sem(self, name=""):
        self.nsem += 1
        h = self.stack.enter_context(self.nc.semaphore(f"s{self.nsem}_{name}"))
        return Sem(h, self.nsem)

    def init_mem(self, arena_f32_cols):
        self.arena = self.stack.enter_context(
            self.nc.sbuf_tensor("arena", [128, arena_f32_cols], F32))
        self.arena_cols = arena_f32_cols
        self.psum = self.stack.enter_context(
            self.nc.psum_tensor("psum", [128, 8 * 512], F32))

    def reset_arena(self, off=0):
        self.arena_off = off

    def alloc(self, cols, dtype=F32, name="", parts=128):
        esz = 4 if dtype in (F32, I32) else 2
        n4 = (cols * esz + 3) // 4
        a = self.arena[0:parts, self.arena_off:self.arena_off + n4]
        self.arena_off += n4
        assert self.arena_off <= self.arena_cols, (name, self.arena_off)
        if dtype != F32:
            a = a.bitcast(dtype)
            if a.shape[-1] != cols:
                a = a[:, 0:cols]
        return Buf(a, name)

    def bank(self, i, dtype=F32, name=""):
        a = self.psum[:, i * 512:(i + 1) * 512]
        if dtype != F32:
            a = a.bitcast(dtype)
        return Buf(a, name or f"bank{i}")

    def _deps(self, reads, writes, extra=()):
        deps = list(extra)
        for b in reads:
            if b.w is not None:
                deps.append(b.w)
        for b in writes:
            if b.w is not None:
                deps.append(b.w)
            deps.extend(b.r)
        return deps

    def _commit(self, tok, reads, writes):
        for b in reads:
            b.r.append(tok)
            if len(b.r) > 64:
                b.r = b.r[-64:]
        for b in writes:
            b.w = tok
            b.r = []

    def _waits(self, eng, deps):
        waits = []
        best = {}
        for t in deps:
            if t is None:
                continue
            s, v = t
            if s.key not in best or best[s.key][1] < v:
                best[s.key] = (s, v)
        for k, (s, v) in best.items():
            if eng == "pe" and s is self.sem.get("pe"):
                continue
            if self.waited[eng].get(k, 0) >= v:
                continue
            self.waited[eng][k] = v
            waits.append((s.handle, v))
        return waits

    def op(self, eng, fn, reads=(), writes=(), extra=()):
        deps = self._deps(reads, writes, extra)
        waits = self._waits(eng, deps)
        self.cnt[eng] += 1
        es = self.sem[eng]
        es.count += 1
        tok = (es, es.count)

        rec = _Rec()
        fn(rec)
        call = rec.call

        def run(e, waits=waits, call=call, h=es.handle):
            for (sh, v) in waits:
                e.wait_ge(sh, v)
            getattr(e, call[0])(*call[1], **call[2]).then_inc(h, 1)

        self.q[eng].append(run)
        self._commit(tok, reads, writes)
        return tok

    def _dsem(self, b, eng="sp"):
        kind = "sw" if eng == "pool" else "hw"
        if b.dsem is None:
            if self.free_dma_sems[kind]:
                b.dsem = self.free_dma_sems[kind].pop()
            else:
                b.dsem = self.new_sem("d" + kind)
            b.dkind = kind
        assert b.dkind == kind, (b.name, b.dkind, kind)
        return b.dsem

    def dma(self, eng, out, in_, reads=(), writes=(), sem_buf=None, extra=(), **kw):
        deps = self._deps(reads, writes, extra)
        waits = self._waits(eng, deps)
        s = self._dsem(sem_buf, eng)
        s.count += 16
        tok = (s, s.count)

        def run(e, waits=waits, h=s.handle, out=out, in_=in_, kw=kw):
            for (sh, v) in waits:
                e.wait_ge(sh, v)
            e.dma_start(out=out, in_=in_, **kw).then_inc(h, 16)

        self.q[eng].append(run)
        self._commit(tok, reads, writes)
        self.dma_toks[s.key] = tok
        return tok

    def raw(self, eng, fn, reads=(), writes=(), extra=(), sem_buf=None):
        deps = self._deps(reads, writes, extra)
        waits = self._waits(eng, deps)
        s = self._dsem(sem_buf, eng)
        s.count += 16
        tok = (s, s.count)

        rec = _Rec()
        fn(rec)
        call = rec.call

        def run(e, waits=waits, h=s.handle, call=call):
            for (sh, v) in waits:
                e.wait_ge(sh, v)
            getattr(e, call[0])(*call[1], **call[2]).then_inc(h, 16)

        self.q[eng].append(run)
        self._commit(tok, reads, writes)
        self.dma_toks[s.key] = tok
        return tok

    def barrier(self):
        toks = []
        for e in ("pe", "act", "dve", "pool"):
            s = self.sem[e]
            if s.count:
                toks.append((s, s.count))
        toks.extend(self.dma_toks.values())
        for eng in ENGS:
            waits = self._waits(eng, toks)
            if waits:
                def run(e, waits=waits):
                    for (sh, v) in waits:
                        e.wait_ge(sh, v)
                self.q[eng].append(run)

    def release_dma_sems(self, bufs):
        for b in bufs:
            if b.dsem is not None:
                self.free_dma_sems[b.dkind].append(b.dsem)
                b.dsem = None

    def emit(self):
        with self.nc.Block() as block:
            @block.tensor
            def _(e):
                for f in self.q["pe"]:
                    f(e)

            @block.scalar
            def _(e):
                for f in self.q["act"]:
                    f(e)

            @block.vector
            def _(e):
                for f in self.q["dve"]:
                    f(e)

            @block.gpsimd
            def _(e):
                for f in self.q["pool"]:
                    f(e)

            @block.sync
            def _(e):
                for f in self.q["sp"]:
                    f(e)
        self.stack.close()

D = 2048
SEQ = 8192
TOWN = 4096
NH = 8
DH = 128
NE = 32
HID = 512
INW = 9216
BIG = 30000.0
NT_E = 48
TSL = 512
NSLOT = NT_E * TSL
EPS = 1e-6


DBG = set()


def dram(nc, name, shape, dt, kind="Internal"):
    if name in DBG and kind == "Internal":
        kind = "ExternalOutput"
    return nc.dram_tensor(name, shape, dt, kind=kind).ap()


def v3(buf, a, b=None):
    return buf.ap.rearrange("p (a b) -> p a b", a=a)


def build(stage=99, dbg=()):
    DBG.clear()
    DBG.update(dbg)
    nc = bass.Bass("TRN2", target_bir_lowering=False)
    I = {}

    def inp(name, shape, dt=F32):
        I[name] = dram(nc, name, shape, dt, "ExternalInput")
        return I[name]

    xa = inp("xa", [SEQ, D])
    xo = inp("xo", [TOWN, D])
    w_in = inp("w_in", [D, INW])
    ga_b = inp("ga_b", [128, D])
    gf_b = inp("gf_b", [128, D])
    gz_b = inp("gz_b", [128, D])
    ident = inp("ident", [128, 128])
    convw = inp("convw", [128, 8 * 4])
    lruv = inp("lruv", [128, 8 * 4])
    lru_wa = inp("lru_wa", [8, 128, 128])
    lru_wx = inp("lru_wx", [8, 128, 128])
    par = inp("par", [128, 2])
    LTt = inp("LTt", [NH, 36, 64 * 128])
    RCt = inp("RCt", [NH, 4, 16 * 256])
    NM2t = inp("NM2t", [128, 8 * 512])
    PBt = inp("PBt", [128, 16 * 32])
    VMt = inp("VMt", [128, 16 * 32])
    proj_rec = inp("proj_rec", [1024, D])
    proj_attn = inp("proj_attn", [1024, D])
    w_out = inp("w_out", [D, D])
    w_r = inp("w_r", [D, 36])
    b_r = inp("b_r", [128, 36])
    ewg = inp("ewg", [NE, D, HID])
    ewu = inp("ewu", [NE, D, HID])
    ewd = inp("ewd", [NE, HID, D])
    tri = inp("tri", [128, 128])
    recinit = inp("recinit", [NSLOT, 4])
    thr96 = inp("thr96", [128, NT_E * 32])
    iot = inp("iot", [128, 32])
    out = dram(nc, "out", [TOWN, D], F32, "ExternalOutput")

    hTa = dram(nc, "hTa", [16, 128, SEQ], BF16)
    hTo = dram(nc, "hTo", [16, 128, TOWN], BF16)
    kTd = dram(nc, "kTd", [8, 128, SEQ], BF16)
    Vd = dram(nc, "Vd", [SEQ, 1024], BF16)
    xrT = dram(nc, "xrT", [8, 128, SEQ], F32)
    qTd = dram(nc, "qTd", [8, 128, TOWN], BF16)
    ggT = dram(nc, "ggT", [8, 128, TOWN], BF16)
    sgT = dram(nc, "sgT", [32, 128, TOWN], BF16)
    yrT = dram(nc, "yrT", [8, 128, TOWN], BF16)
    yaT = dram(nc, "yaT", [8, 128, TOWN], BF16)
    mTd = dram(nc, "mTd", [16, 128, TOWN], BF16)
    x2d = dram(nc, "x2d", [TOWN, D], F32)
    x2n = dram(nc, "x2n", [TOWN + 128, D], BF16)
    recs = dram(nc, "recs", [NSLOT, 4], F32)
    ybuf = dram(nc, "ybuf", [2 * TOWN + 128, D], F32)
    ewgb = dram(nc, "ewgb", [NE, D, HID], BF16)
    ewub = dram(nc, "ewub", [NE, D, HID], BF16)
    ewdb = dram(nc, "ewdb", [NE, HID, D], BF16)
    precast_bufs = [Buf(None, f"pcb{i}") for i in range(4)]
    precast_n = [0]

    def emit_precast(e_idx):
        for (src, dst) in ((ewg, ewgb), (ewu, ewub), (ewd, ewdb)):
            pb_ = precast_bufs[precast_n[0] % 4]
            precast_n[0] += 1
            P.dma("pool", dst[e_idx].rearrange("(p r) n -> p (r n)", p=128), src[e_idx].rearrange("(p r) n -> p (r n)", p=128),
                  sem_buf=pb_)

    dbg_out = {}

    P = Prog(nc)
    P.init_mem(47 * 1024)

    idb = P.alloc(128, BF16, "idb")
    idf = P.alloc(128, F32, "idf")
    onesb = P.alloc(128, BF16, "onesb")
    epsb = P.alloc(1, F32, "epsb")
    parb = P.alloc(2, F32, "parb")
    P.dma("pool", idb[:, :], ident[:, :], writes=[idb], sem_buf=idb)
    P.dma("sp", idf[:, :], ident[:, :], writes=[idf], sem_buf=idf)
    P.dma("sp", parb[:, :], par[:, :], writes=[parb], sem_buf=parb)
    P.op("dve", lambda e: e.memset(onesb[:, :], 1.0), writes=[onesb])
    P.op("dve", lambda e: e.memset(epsb[:, :], EPS), writes=[epsb])
    iob = P.alloc(32, F32, "iob")
    P.dma("sp", iob[:, :], iot[:, :], writes=[iob], sem_buf=iob)
    te = P.alloc(NT_E, F32, "te")
    base_off = P.arena_off

    def norm_rows(xs_ap, xs_buf, gb, xn_b, ss_b, rs_b, sq):
        P.op("act", lambda e: e.activation(out=sq[:, :], in_=xs_ap, func=AF.Square, accum_out=ss_b[:, :]),
             reads=[xs_buf], writes=[sq, ss_b])
        P.op("act", lambda e: e.activation(out=rs_b[:, :], in_=ss_b[:, :], func=AF.Sqrt, scale=1.0 / D, bias=epsb[:, :]),
             reads=[ss_b, epsb], writes=[rs_b])
        P.op("dve", lambda e: e.reciprocal(out=rs_b[:, :], in_=rs_b[:, :]), reads=[rs_b], writes=[rs_b])
        P.op("dve", lambda e: e.scalar_tensor_tensor(out=xn_b[:, :], in0=xs_ap, scalar=rs_b[:, 0:1], in1=gb[:, :],
                                                     op0=ALU.mult, op1=ALU.mult),
             reads=[xs_buf, rs_b, gb], writes=[xn_b])

    def transpose_rows(xn_b, dst_ap_fn, dst_buf, pbank, eng_pair=("act", "dve"), strided=False):
        for hlf in range(2):
            pb = pbank[hlf]
            for kk in range(8):
                k = hlf * 8 + kk
                src_cols = (xn_b.ap.rearrange("s (p j) -> s j p", j=16)[:, k, :] if strided else xn_b[:, k * 128:(k + 1) * 128])
                P.op("pe", lambda e, k=k, kk=kk, pb=pb: e.transpose(out=pb[:, kk * 128:(kk + 1) * 128],
                                                                   in_=src_cols, identity=idb[:, :]),
                     reads=[xn_b, idb], writes=[pb])
            eng = eng_pair[hlf]
            src = pb.ap.rearrange("p (a b) -> p a b", a=8)
            if eng == "act":
                P.op("act", lambda e, src=src, hlf=hlf: e.activation(out=dst_ap_fn(hlf), in_=src, func=AF.Copy),
                     reads=[pb], writes=[dst_buf])
            else:
                P.op("dve", lambda e, src=src, hlf=hlf: e.tensor_copy(out=dst_ap_fn(hlf), in_=src),
                     reads=[pb], writes=[dst_buf])

    def phase1(x_dram, T, hT_dram):
        P.reset_arena(base_off)
        gb = P.alloc(D, F32, "gb")
        P.dma("sp", gb[:, :], ga_b[:, :], writes=[gb], sem_buf=gb)
        xin = [P.alloc(4 * D, F32, f"xin{i}") for i in range(2)]
        sq = P.alloc(D, F32, "sq")
        xn = [P.alloc(D, BF16, f"xn{i}") for i in range(2)]
        ss = [P.alloc(1, F32) for i in range(2)]
        rs = [P.alloc(1, F32) for i in range(2)]
        hT = [P.alloc(16 * 512, BF16, f"hT{i}") for i in range(2)]
        pbanks = [(P.bank(0, BF16), P.bank(1, BF16)), (P.bank(2, BF16), P.bank(3, BF16))]
        for g in range(T // 512):
            xb = xin[g % 2]
            P.dma("sp", xb.ap.rearrange("p (j d) -> p j d", j=4),
                  x_dram[g * 512:(g + 1) * 512, :].rearrange("(j p) d -> p j d", p=128), writes=[xb], sem_buf=xb)
            hb = hT[g % 2]
            hv = hb.ap.rearrange("p (k t) -> p k t", k=16)
            for j in range(4):
                i = g * 4 + j
                norm_rows(xb.ap[:, j * D:(j + 1) * D], xb, gb, xn[i % 2], ss[i % 2], rs[i % 2], sq)
                transpose_rows(xn[i % 2], lambda hlf, j=j, hv=hv: hv[:, hlf * 8:(hlf + 1) * 8, j * 128:(j + 1) * 128],
                               hb, pbanks[i % 2])
            P.dma("act", hT_dram[:, :, g * 512:(g + 1) * 512].rearrange("k p t -> p k t"), hv, reads=[hb], sem_buf=hb)
        P.barrier()
        P.release_dma_sems([gb] + xin + hT)

    phase1(xa, SEQ, hTa)
    phase1(xo, TOWN, hTo)

    def gelu_evac(pb, ob_ap, ob, tmp1, tmp2):
        P.op("act", lambda e: e.activation(out=tmp1[:, :], in_=pb[:, :], func=AF.Square), reads=[pb], writes=[tmp1])
        P.op("dve", lambda e: e.tensor_scalar(out=tmp1[:, :], in0=tmp1[:, :], scalar1=0.044715, scalar2=1.0,
                                              op0=ALU.mult, op1=ALU.add), reads=[tmp1], writes=[tmp1])
        P.op("dve", lambda e: e.tensor_tensor(out=tmp1[:, :], in0=tmp1[:, :], in1=pb[:, :], op=ALU.mult),
             reads=[tmp1, pb], writes=[tmp1])
        P.op("act", lambda e: e.activation(out=tmp2[:, :], in_=tmp1[:, :], func=AF.Sigmoid, scale=1.5957691216),
             reads=[tmp1], writes=[tmp2])
        P.op("dve", lambda e: e.tensor_tensor(out=ob_ap, in0=tmp2[:, :], in1=pb[:, :], op=ALU.mult),
             reads=[tmp2, pb], writes=[ob])

    def inproj(hT_dram, T, groups):
        P.reset_arena(base_off)
        wt = [P.alloc(16 * 512, BF16, f"wt{i}") for i in range(2)]
        ht = [P.alloc(16 * 512, BF16, f"ht{i}") for i in range(3)]
        ob = [P.alloc(4 * 512, BF16, f"ob{i}") for i in range(2)]
        obf = [P.alloc(4 * 512, F32, f"obf{i}") for i in range(2)]
        tmp1 = P.alloc(512, F32, "tmp1")
        tmp2 = P.alloc(512, F32, "tmp2")
        banks = [P.bank(i) for i in range(4)]
        ntg = T // 512
        it = 0
        nb = 0
        for gi, (col0, kind, dst, dchunk0) in enumerate(groups):
            w = wt[gi % 2]
            wv = w.ap.rearrange("p (k n) -> p k n", k=16)
            P.dma("pool", wv, w_in[:, col0:col0 + 512].rearrange("(k p) n -> p k n", p=128), writes=[w], sem_buf=w)
            for tg in range(ntg):
                h = ht[it % 3]
                hv = h.ap.rearrange("p (k t) -> p k t", k=16)
                P.dma("sp", hv, hT_dram[:, :, tg * 512:(tg + 1) * 512].rearrange("k p t -> p k t"), writes=[h], sem_buf=h)
                fp = kind == "xr"
                o = (obf if fp else ob)[it % 2]
                ov = o.ap.rearrange("p (c t) -> p c t", c=4)
                for cc in range(4):
                    pb = banks[nb % 4]
                    nb += 1
                    for k in range(16):
                        if kind == "v":
                            P.op("pe", lambda e, k=k, cc=cc, pb=pb, hv=hv, wv=wv: e.matmul(
                                pb[:, :], lhsT=hv[:, k, cc * 128:(cc + 1) * 128], rhs=wv[:, k, :], start=(k == 0), stop=(k == 15)),
                                reads=[h, w], writes=[pb])
                        else:
                            P.op("pe", lambda e, k=k, cc=cc, pb=pb, hv=hv, wv=wv: e.matmul(
                                pb[:, :], lhsT=wv[:, k, cc * 128:(cc + 1) * 128], rhs=hv[:, k, :], start=(k == 0), stop=(k == 15)),
                                reads=[h, w], writes=[pb])
                    oa = ov[:, cc, :]
                    if kind == "gg":
                        gelu_evac(pb, oa, o, tmp1, tmp2)
                    elif kind == "sg":
                        P.op("act", lambda e, oa=oa, pb=pb: e.activation(out=oa, in_=pb[:, :], func=AF.Sigmoid),
                             reads=[pb], writes=[o])
                    elif kind == "q":
                        P.op("act", lambda e, oa=oa, pb=pb: e.activation(out=oa, in_=pb[:, :], func=AF.Copy, scale=DH ** -0.5),
                             reads=[pb], writes=[o])
                    elif cc % 2 == 0:
                        P.op("act", lambda e, oa=oa, pb=pb: e.activation(out=oa, in_=pb[:, :], func=AF.Copy),
                             reads=[pb], writes=[o])
                    else:
                        P.op("dve", lambda e, oa=oa, pb=pb: e.tensor_copy(out=oa, in_=pb[:, :]), reads=[pb], writes=[o])
                if kind == "v":
                    P.dma("act", dst[tg * 512:(tg + 1) * 512, dchunk0 * 128:dchunk0 * 128 + 512].rearrange("(c p) n -> p c n", p=128),
                          ov, reads=[o], sem_buf=o)
                else:
                    P.dma("act", dst[dchunk0:dchunk0 + 4, :, tg * 512:(tg + 1) * 512].rearrange("c p t -> p c t"),
                          ov, reads=[o], sem_buf=o)
                it += 1
        P.barrier()
        P.release_dma_sems(wt + ht + ob + obf)

    if stage >= 2:
        grpA = []
        for j in range(2):
            grpA.append((0 + j * 512, "xr", xrT, j * 4))
        for j in range(2):
            grpA.append((3072 + j * 512, "k", kTd, j * 4))
        for j in range(2):
            grpA.append((4096 + j * 512, "v", Vd, j * 4))
        inproj(hTa, SEQ, grpA)
        grpO = []
        for j in range(2):
            grpO.append((1024 + j * 512, "gg", ggT, j * 4))
        for j in range(2):
            grpO.append((2048 + j * 512, "q", qTd, j * 4))
        for j in range(8):
            grpO.append((5120 + j * 512, "sg", sgT, j * 4))
        inproj(hTo, TOWN, grpO)

    lru_rel = []

    def lru_groups(standalone):
        if standalone:
            P.reset_arena(base_off)
        cw = P.alloc(32, F32, "cw")
        lv = P.alloc(32, F32, "lv")
        c8 = P.alloc(8, F32, "c8")
        P.dma("sp", cw[:, :], convw[:, :], writes=[cw], sem_buf=cw)
        P.dma("sp", lv[:, :], lruv[:, :], writes=[lv], sem_buf=lv)
        lvv = lv.ap.rearrange("p (n f) -> p n f", n=8)
        P.op("act", lambda e: e.activation(out=c8[:, :], in_=lvv[:, :, 3], func=AF.Exp, scale=-1.0), reads=[lv], writes=[c8])
        P.op("dve", lambda e: e.tensor_scalar(out=c8[:, :], in0=c8[:, :], scalar1=1.0, scalar2=None, op0=ALU.add),
             reads=[c8], writes=[c8])
        P.op("act", lambda e: e.activation(out=c8[:, :], in_=c8[:, :], func=AF.Ln), reads=[c8], writes=[c8])
        P.op("dve", lambda e: e.tensor_scalar(out=c8[:, :], in0=c8[:, :], scalar1=-8.0, scalar2=None, op0=ALU.mult),
             reads=[c8], writes=[c8])
        wab = [P.alloc(128, BF16, f"wa{i}") for i in range(8)]
        wxb = [P.alloc(128, BF16, f"wx{i}") for i in range(8)]
        LR = 4
        xr = [P.alloc(515, F32, f"xr{i}") for i in range(LR)]
        gg = [P.alloc(256, BF16, f"gg{i}") for i in range(LR)]
        for n_ in range(8):
            P.dma("pool", wab[n_][:, :], lru_wa[n_], writes=[wab[n_]], sem_buf=wab[n_])
            P.dma("pool", wxb[n_][:, :], lru_wx[n_], writes=[wxb[n_]], sem_buf=wxb[n_])

        def lru_loads(i):
            n_, tg_ = i // 16, i % 16
            x = xr[i % LR]
            if tg_ == 0:
                P.op("dve", lambda e: e.memset(x[:, 0:3], 0.0), writes=[x])
                P.dma("sp", x[:, 3:515], xrT[n_, :, 0:512], writes=[x], sem_buf=x)
            else:
                P.dma("sp", x[:, 0:515], xrT[n_, :, tg_ * 512 - 3:tg_ * 512 + 512], writes=[x], sem_buf=x)
            g_ = gg[i % LR]
            P.dma("sp", g_[:, :], ggT[n_, :, tg_ * 256:(tg_ + 1) * 256], writes=[g_], sem_buf=g_)

        for i_ in range(LR - 1):
            lru_loads(i_)
        xc = P.alloc(512, F32, "xc")
        xcb = P.alloc(512, BF16, "xcb")
        rr = P.alloc(512, F32, "rr")
        ii = P.alloc(512, F32, "ii")
        aa = P.alloc(512, F32, "aa")
        a2 = P.alloc(512, F32, "a2")
        bb = P.alloc(512, F32, "bb")
        hh = [P.alloc(512, F32, f"hh{i}") for i in range(2)]
        h0 = P.alloc(1, F32, "h0")
        ysel = P.alloc(256, F32, "ysel")
        yo = [P.alloc(256, BF16, f"yo{i}") for i in range(2)]
        P.op("dve", lambda e: e.memset(h0[:, :], 0.0), writes=[h0])
        if standalone:
            bA, bB = P.bank(4), P.bank(5)
        else:
            bA, bB = P.bank(6), P.bank(2)
        xc2 = [xc, P.alloc(512, F32, "xc_b")]
        xcb2 = [xcb, P.alloc(512, BF16, "xcb_b")]

        def stage1(i):
            n, tg = divmod(i, 16)
            x = xr[i % LR]
            xc_ = xc2[i % 2]
            xcb_ = xcb2[i % 2]
            P.op("dve", lambda e: e.tensor_scalar(out=xc_[:, :], in0=x[:, 0:512], scalar1=cw[:, n * 4:n * 4 + 1],
                                                  scalar2=lvv[:, n, 0:1], op0=ALU.mult, op1=ALU.add),
                 reads=[x, cw, lv], writes=[xc_])
            for k in range(1, 4):
                P.op("dve", lambda e: e.scalar_tensor_tensor(
                    out=xc_[:, :], in0=x[:, k:k + 512], scalar=cw[:, n * 4 + k:n * 4 + k + 1], in1=xc_[:, :],
                    op0=ALU.mult, op1=ALU.add), reads=[x, cw, xc_], writes=[xc_])
            P.op("act", lambda e: e.activation(out=xcb_[:, :], in_=xc_[:, :], func=AF.Copy), reads=[xc_], writes=[xcb_])

        def stage2(i):
            n, tg = divmod(i, 16)
            wa, wx = wab[n], wxb[n]
            xc_ = xc2[i % 2]
            xcb_ = xcb2[i % 2]
            g_ = gg[i % LR]
            P.op("pe", lambda e: e.matmul(bA[:, :], lhsT=wa[:, :], rhs=xcb_[:, :], start=True, stop=True),
                 reads=[wa, xcb_], writes=[bA])
            P.op("pe", lambda e: e.matmul(bB[:, :], lhsT=wx[:, :], rhs=xcb_[:, :], start=True, stop=True),
                 reads=[wx, xcb_], writes=[bB])
            P.op("act", lambda e: e.activation(out=rr[:, :], in_=bA[:, :], func=AF.Sigmoid, bias=lvv[:, n, 1:2]),
                 reads=[bA, lv], writes=[rr])
            P.op("act", lambda e: e.activation(out=ii[:, :], in_=bB[:, :], func=AF.Sigmoid, bias=lvv[:, n, 2:3]),
                 reads=[bB, lv], writes=[ii])
            P.op("act", lambda e: e.activation(out=aa[:, :], in_=rr[:, :], func=AF.Exp, scale=c8[:, n:n + 1]),
                 reads=[rr, c8], writes=[aa])
            P.op("dve", lambda e: e.tensor_tensor(out=a2[:, :], in0=aa[:, :], in1=aa[:, :], op=ALU.mult),
                 reads=[aa], writes=[a2])
            P.op("dve", lambda e: e.tensor_scalar(out=a2[:, :], in0=a2[:, :], scalar1=-1.0, scalar2=1.0,
                                                  op0=ALU.mult, op1=ALU.add), reads=[a2], writes=[a2])
            P.op("act", lambda e: e.activation(out=a2[:, :], in_=a2[:, :], func=AF.Sqrt), reads=[a2], writes=[a2])
            P.op("dve", lambda e: e.tensor_tensor(out=bb[:, :], in0=ii[:, :], in1=xc_[:, :], op=ALU.mult),
                 reads=[ii, xc_], writes=[bb])
            P.op("dve", lambda e: e.tensor_tensor(out=bb[:, :], in0=bb[:, :], in1=a2[:, :], op=ALU.mult),
                 reads=[bb, a2], writes=[bb])
            hcur = hh[i % 2]
            hprev = hh[(i + 1) % 2]
            if tg == 0:
                P.op("dve", lambda e: e.tensor_tensor_scan(out=hcur[:, :], data0=aa[:, :], data1=bb[:, :],
                                                           initial=h0[:, 0:1], op0=ALU.mult, op1=ALU.add),
                     reads=[aa, bb, h0], writes=[hcur])
            else:
                P.op("dve", lambda e: e.tensor_tensor_scan(
                    out=hcur[:, :], data0=aa[:, :], data1=bb[:, :], initial=hprev[:, 511:512], op0=ALU.mult, op1=ALU.add),
                    reads=[aa, bb, hprev], writes=[hcur])
            P.op("dve", lambda e: e.tensor_scalar(out=ysel[:, :], in0=hcur[:, 0:256], scalar1=parb[:, 0:1],
                                                  scalar2=None, op0=ALU.mult), reads=[hcur, parb], writes=[ysel])
            P.op("dve", lambda e: e.scalar_tensor_tensor(out=ysel[:, :], in0=hcur[:, 256:512], scalar=parb[:, 1:2],
                                                         in1=ysel[:, :], op0=ALU.mult, op1=ALU.add),
                 reads=[hcur, parb, ysel], writes=[ysel])
            y = yo[i % 2]
            P.op("dve", lambda e: e.tensor_tensor(out=y[:, :], in0=ysel[:, :], in1=g_[:, :], op=ALU.mult),
                 reads=[ysel, g_], writes=[y])
            P.dma("act", yrT[n, :, tg * 256:(tg + 1) * 256], y[:, :], reads=[y], sem_buf=y)

        stage1(0)
        for i in range(128):
            if i + LR - 1 < 128:
                lru_loads(i + LR - 1)
            if i + 1 < 128:
                stage1(i + 1)
            stage2(i)
            yield
        lru_rel.extend([cw, lv] + wab + wxb + xr + gg + yo)

    def phase_lru():
        for _ in lru_groups(True):
            pass
        P.barrier()
        P.release_dma_sems(lru_rel)

    if stage == 3:
        phase_lru()

    def phase_attn():
        P.reset_arena(base_off)
        kTs = [P.alloc(SEQ, BF16, f"kT{i}") for i in range(2)]
        Vhs = [P.alloc(64 * 128, BF16, f"Vh{i}") for i in range(2)]
        qTs = [P.alloc(TOWN, BF16, f"qT{i}") for i in range(2)]
        LTs = [P.alloc(64 * 128, BF16, f"LT{i}", parts=36) for i in range(2)]
        RBs = [P.alloc(16 * 256, BF16, f"RB{i}", parts=36) for i in range(2)]
        RBcs = [Buf(None, f"RBc{i}") for i in range(2)]
        NM = P.alloc(8 * 512, BF16, "NM")
        PB = P.alloc(16 * 32, F32, "PB")
        VM = P.alloc(16 * 32, F32, "VM")
        km = P.alloc(32, F32, "km")
        kmb = P.alloc(32, BF16, "kmb")
        gm = P.alloc(32, F32, "gm")
        t8 = P.alloc(8, F32, "t8")
        PT = [P.alloc(512, BF16, f"PT{i}") for i in range(3)]
        rec = P.alloc(512, F32, "rec")
        yo = [P.alloc(512, BF16, f"ya{i}") for i in range(2)]
        P.dma("pool", NM[:, :], NM2t[:, :], writes=[NM], sem_buf=NM)
        P.dma("sp", PB[:, :], PBt[:, :], writes=[PB], sem_buf=PB)
        P.dma("sp", VM[:, :], VMt[:, :], writes=[VM], sem_buf=VM)
        Sb = [P.bank(i) for i in range(2)]
        OTb = P.bank(3)
        SMb = P.bank(4)
        Gb = P.bank(5)
        Tb = P.bank(7)
        PBv = PB.ap.rearrange("p (s n) -> p s n", s=16)
        VMv = VM.ap.rearrange("p (s n) -> p s n", s=16)
        NMv = NM.ap.rearrange("p (c q) -> p c q", c=8)
        cnt_ = {"pti": 0, "sbi": 0, "chunk": 0, "selgen": None}
        lru = lru_groups(False)

        def emit_load(h):
            kT, Vh, qT, LT, RB, RBc = kTs[h % 2], Vhs[h % 2], qTs[h % 2], LTs[h % 2], RBs[h % 2], RBcs[h % 2]
            Vv = Vh.ap.rearrange("p (c d) -> p c d", c=64)
            RBv = RB.ap.rearrange("p (s q) -> p s q", s=16)
            P.dma("sp", kT[:, :], kTd[h], writes=[kT], sem_buf=kT)
            P.dma("sp", qT[:, :], qTd[h], writes=[qT], sem_buf=qT)
            P.dma("sp", Vv, Vd[:, h * 128:(h + 1) * 128].rearrange("(c p) d -> p c d", p=128), writes=[Vh], sem_buf=Vh)
            P.dma("pool", LT[:, :], LTt[h], writes=[LT], sem_buf=LT)
            P.dma("pool", RBv[32:36, :, :], RCt[h].rearrange("r (s q) -> r s q", s=16), reads=[], writes=[RB], sem_buf=RBc)
            for e_idx in range(4 * h, 4 * h + 4):
                emit_precast(e_idx)

        sel2 = [P.alloc(2 * 32, F32, f"sel2_{i}") for i in range(2)]

        def sel_steps(h):
            kT, qT, RB = kTs[h % 2], qTs[h % 2], RBs[h % 2]
            RBv = RB.ap.rearrange("p (s q) -> p s q", s=16)
            P.op("dve", lambda e: e.tensor_reduce(out=km[:, :], in_=kT.ap.rearrange("p (n k) -> p n k", n=32), axis=AX.X, op=ALU.add),
                 reads=[kT], writes=[km])
            P.op("dve", lambda e: e.tensor_scalar(out=kmb[:, :], in0=km[:, :], scalar1=1.0 / 256, scalar2=None, op0=ALU.mult),
                 reads=[km], writes=[kmb])
            yield

            def gating(hq):
                for qq in range(16):
                    qt = hq * 16 + qq
                    P.op("pe", lambda e: e.matmul(Gb[:, qq * 32:qq * 32 + 32], lhsT=qT[:, qt * 128:(qt + 1) * 128], rhs=kmb[:, :],
                                                  start=True, stop=True), reads=[qT, kmb], writes=[Gb])

            def stepA(s_):
                sb_ = sel2[s_ % 2]
                sv = sb_.ap.rearrange("p (a n) -> p a n", a=2)
                for half in range(2):
                    qq = (2 * s_ + half) % 16
                    gsl = Gb[:, qq * 32:qq * 32 + 32]
                    sl = sv[:, half, :]
                    P.op("dve", lambda e: e.tensor_tensor(out=gm[:, :], in0=gsl, in1=PBv[:, s_, :], op=ALU.add),
                         reads=[Gb, PB], writes=[gm])
                    P.op("dve", lambda e: e.max(out=t8[:, :], in_=gm[:, :]), reads=[gm], writes=[t8])
                    P.op("dve", lambda e: e.tensor_scalar(out=sl, in0=gm[:, :], scalar1=t8[:, 2:3], scalar2=-1.0,
                                                          op0=ALU.is_ge, op1=ALU.add), reads=[gm, t8], writes=[sb_])
                    P.op("dve", lambda e: e.tensor_tensor(out=sl, in0=sl, in1=VMv[:, s_, :], op=ALU.mult),
                         reads=[sb_, VM], writes=[sb_])

            def stepB(s_):
                sb_ = sel2[s_ % 2]
                sv = sb_.ap.rearrange("p (a n) -> p a n", a=2)
                for half in range(2):
                    P.op("pe", lambda e: e.transpose(out=Tb[0:32, half * 128:(half + 1) * 128], in_=sv[:, half, :], identity=idf[:, :]),
                         reads=[sb_, idf], writes=[Tb])
                P.op("act", lambda e: e.activation(out=RBv[0:32, s_, :], in_=Tb[0:32, 0:256], func=AF.Copy),
                     reads=[Tb], writes=[RB])

            for hq in range(2):
                gating(hq)
                yield
                for i in range(8):
                    s_ = hq * 8 + i
                    stepA(s_)
                    if i > 0:
                        stepB(s_ - 1)
                    yield
                stepB(hq * 8 + 7)
                yield

        def emit_pair(h, sp):
            kT, Vh, qT, LT, RB = kTs[h % 2], Vhs[h % 2], qTs[h % 2], LTs[h % 2], RBs[h % 2]
            Vv = Vh.ap.rearrange("p (c d) -> p c d", c=64)
            LTv = LT.ap.rearrange("p (c k) -> p c k", c=64)
            nch = 8 * sp + 8
            s0 = 2 * sp
            qs = qT[:, sp * 512:(sp + 1) * 512]
            rb = RB[0:36, s0 * 256:(s0 + 2) * 256]

            def emit_S(c):
                sb = Sb[cnt_["sbi"] % 2]
                cnt_["sbi"] += 1
                near = c >= 8 * sp
                P.op("pe", lambda e: e.matmul(sb[:, :], lhsT=kT[:, c * 128:(c + 1) * 128], rhs=qs, start=True, stop=False),
                     reads=[kT, qT], writes=[sb])
                P.op("pe", lambda e: e.matmul(sb[:, :], lhsT=LTv[0:36, c, :], rhs=rb, start=False, stop=(not near)),
                     reads=[LT, RB], writes=[sb])
                if near:
                    P.op("pe", lambda e: e.matmul(sb[:, :], lhsT=idb[:, :], rhs=NMv[:, c - 8 * sp, :], start=False, stop=True),
                         reads=[idb, NM], writes=[sb])
                return sb

            def emit_exp(sb):
                pt = PT[cnt_["pti"] % 3]
                cnt_["pti"] += 1
                P.op("act", lambda e: e.activation(out=pt[:, :], in_=sb[:, :], func=AF.Exp), reads=[sb], writes=[pt])
                return pt

            def emit_PV(c, pt):
                P.op("pe", lambda e: e.matmul(OTb[:, :], lhsT=Vv[:, c, :], rhs=pt[:, :], start=(c == 0), stop=(c == nch - 1)),
                     reads=[Vh, pt], writes=[OTb])
                P.op("pe", lambda e: e.matmul(SMb[:, :], lhsT=onesb[:, :], rhs=pt[:, :], start=(c == 0), stop=(c == nch - 1)),
                     reads=[onesb, pt], writes=[SMb])

            sbs = [emit_S(0)]
            for c in range(nch):
                if c + 1 < nch:
                    sbs.append(emit_S(c + 1))
                pt = emit_exp(sbs[c])
                emit_PV(c, pt)
                cnt_["chunk"] += 1
                if cnt_["selgen"] is not None and cnt_["chunk"] % 6 == 0:
                    next(cnt_["selgen"], None)
            y = yo[sp % 2]
            P.op("dve", lambda e: e.reciprocal(out=rec[:, :], in_=SMb[:, :]), reads=[SMb], writes=[rec])
            P.op("dve", lambda e: e.tensor_tensor(out=y[:, :], in0=OTb[:, :], in1=rec[:, :], op=ALU.mult),
                 reads=[OTb, rec], writes=[y])
            P.dma("act", yaT[h, :, sp * 512:(sp + 1) * 512], y[:, :], reads=[y], sem_buf=y)

        emit_load(0)
        emit_load(1)
        for _ in sel_steps(0):
            pass
        for h in range(NH):
            cnt_["selgen"] = sel_steps(h + 1) if h + 1 < NH else None
            for sp in range(8):
                emit_pair(h, sp)
                for _ in range(2):
                    next(lru, None)
            if cnt_["selgen"] is not None:
                for _ in cnt_["selgen"]:
                    pass
            if h + 2 < NH:
                emit_load(h + 2)
        for _ in lru:
            pass
        P.barrier()
        P.release_dma_sems(kTs + Vhs + qTs + LTs + RBcs + [NM, PB, VM] + yo + lru_rel)

    if stage >= 4:
        phase_attn()

    def phase_merge():
        P.reset_arena(base_off)
        pr = P.alloc(8 * D, BF16, "pr")
        pa = P.alloc(8 * D, BF16, "pa")
        prv = pr.ap.rearrange("p (k n) -> p k n", k=8)
        pav = pa.ap.rearrange("p (k n) -> p k n", k=8)
        P.dma("pool", prv, proj_rec.rearrange("(k p) n -> p k n", p=128), writes=[pr], sem_buf=pr)
        P.dma("pool", pav, proj_attn.rearrange("(k p) n -> p k n", p=128), writes=[pa], sem_buf=pa)
        yr = [P.alloc(8 * 512, BF16, f"yr{i}") for i in range(2)]
        ya = [P.alloc(8 * 512, BF16, f"yat{i}") for i in range(2)]
        sg = [P.alloc(32 * 512, BF16, f"sg{i}") for i in range(1)]
        mt = [P.alloc(16 * 512, BF16, f"mt{i}") for i in range(1)]
        t1 = P.alloc(512, F32, "t1")
        t2 = P.alloc(512, F32, "t2")
        banks = [P.bank(i) for i in range(4)]
        nb = 0
        for tg in range(8):
            a, b, g_, m = yr[tg % 2], ya[tg % 2], sg[0], mt[0]
            av = a.ap.rearrange("p (k t) -> p k t", k=8)
            bv = b.ap.rearrange("p (k t) -> p k t", k=8)
            gv = g_.ap.rearrange("p (k t) -> p k t", k=32)
            mv = m.ap.rearrange("p (k t) -> p k t", k=16)
            sl = slice(tg * 512, (tg + 1) * 512)
            P.dma("sp", av, yrT[:, :, sl].rearrange("k p t -> p k t"), writes=[a], sem_buf=a)
            P.dma("sp", bv, yaT[:, :, sl].rearrange("k p t -> p k t"), writes=[b], sem_buf=b)
            P.dma("sp", gv, sgT[:, :, sl].rearrange("k p t -> p k t"), writes=[g_], sem_buf=g_)
            for dc in range(16):
                bR = banks[nb % 4]
                bA_ = banks[(nb + 1) % 4]
                nb += 2
                for k in range(8):
                    P.op("pe", lambda e, k=k, dc=dc, bR=bR, av=av: e.matmul(bR[:, :], lhsT=prv[:, k, dc * 128:(dc + 1) * 128], rhs=av[:, k, :],
                                                                        start=(k == 0), stop=(k == 7)), reads=[pr, a], writes=[bR])
                for k in range(8):
                    P.op("pe", lambda e, k=k, dc=dc, bA_=bA_, bv=bv: e.matmul(bA_[:, :], lhsT=pav[:, k, dc * 128:(dc + 1) * 128], rhs=bv[:, k, :],
                                                                          start=(k == 0), stop=(k == 7)), reads=[pa, b], writes=[bA_])
                P.op("dve", lambda e, bR=bR, gv=gv, dc=dc: e.tensor_tensor(out=t1[:, :], in0=bR[:, :], in1=gv[:, dc, :], op=ALU.mult),
                     reads=[bR, g_], writes=[t1])
                P.op("dve", lambda e, bA_=bA_, gv=gv, dc=dc: e.tensor_tensor(out=t2[:, :], in0=bA_[:, :], in1=gv[:, 16 + dc, :], op=ALU.mult),
                     reads=[bA_, g_], writes=[t2])
                P.op("pool", lambda e, mv=mv, dc=dc: e.tensor_tensor(out=mv[:, dc, :], in0=t1[:, :], in1=t2[:, :], op=ALU.add),
                     reads=[t1, t2], writes=[m])
            P.dma("act", mTd[:, :, sl].rearrange("k p t -> p k t"), mv, reads=[m], sem_buf=m)
        P.barrier()
        P.release_dma_sems([pr, pa] + yr + ya + sg + mt)

    if stage >= 5:
        phase_merge()

    def phase_wout():
        P.reset_arena(base_off)
        zrow = P.alloc(D, BF16, "zrow")
        P.op("pool", lambda e: e.memset(zrow[:, :], 0.0), writes=[zrow])
        P.dma("sp", x2n[TOWN:TOWN + 128, :], zrow[:, :], reads=[zrow], sem_buf=zrow)
        wo = P.alloc(16 * D, BF16, "wo")
        wov = wo.ap.rearrange("p (k n) -> p k n", k=16)
        P.dma("pool", wov, w_out.rearrange("(k p) n -> p k n", p=128), writes=[wo], sem_buf=wo)
        gb = P.alloc(D, F32, "gbf")
        P.dma("sp", gb[:, :], gf_b[:, :], writes=[gb], sem_buf=gb)
        wr = P.alloc(16 * 36, BF16, "wr")
        wrv = wr.ap.rearrange("p (k n) -> p k n", k=16)
        P.dma("pool", wrv, w_r.rearrange("(k p) n -> p k n", p=128), writes=[wr], sem_buf=wr)
        br = P.alloc(36, F32, "br")
        P.dma("sp", br[:, :], b_r[:, :], writes=[br], sem_buf=br)
        trib = P.alloc(128, BF16, "trib")
        P.dma("pool", trib[:, :], tri[:, :], writes=[trib], sem_buf=trib)
        mt = [P.alloc(16 * 512, BF16, f"mtl{i}") for i in range(1)]
        xt = [P.alloc(D, F32, f"xt{i}") for i in range(1)]
        x2 = [P.alloc(D, F32, f"x2{i}") for i in range(2)]
        sq = P.alloc(D, F32, "sq6")
        xn = [P.alloc(D, BF16, f"xn6{i}") for i in range(2)]
        xnT = [P.alloc(16 * 128, BF16, f"xnT{i}") for i in range(2)]
        ss = [P.alloc(1, F32) for i in range(2)]
        rs = [P.alloc(1, F32) for i in range(2)]
        selall = P.alloc(32 * 32, BF16, "selall")
        sel1 = P.alloc(32 * 32, F32, "sel1")
        sel2 = P.alloc(32 * 32, F32, "sel2")
        wgt = P.alloc(32 * 2, F32, "wgt")
        posall = P.alloc(32 * 32, F32, "posall")
        lg = P.alloc(36, F32, "lg")
        gmax = P.alloc(1, F32, "gmax")
        ngmax = P.alloc(1, F32, "ngmax")
        gex = P.alloc(4, F32, "gex")
        gsum = P.alloc(1, F32, "gsum")
        gw = P.alloc(1, F32, "gw")
        gmk = P.alloc(4, F32, "gmk")
        lem = P.alloc(32, F32, "lem")
        m8 = P.alloc(8, F32, "m8")
        dd = P.alloc(1, F32, "dd")
        banks = [P.bank(i) for i in range(4)]
        pbanks = [(P.bank(4, BF16), P.bank(5, BF16))]
        bL = P.bank(6)
        bC = P.bank(7)
        s1v = sel1.ap.rearrange("p (t e) -> p t e", t=32)
        s2v = sel2.ap.rearrange("p (t e) -> p t e", t=32)
        sav = selall.ap.rearrange("p (t e) -> p t e", t=32)
        posv = posall.ap.rearrange("p (t e) -> p t e", t=32)
        wv_ = wgt.ap.rearrange("p (t k) -> p t k", t=32)
        nb = 0
        for tt in range(32):
            tg, j = tt // 4, tt % 4
            m = mt[0]
            mv = m.ap.rearrange("p (k t) -> p k t", k=16)
            if j == 0:
                P.dma("sp", mv, mTd[:, :, tg * 512:(tg + 1) * 512].rearrange("k p t -> p k t"), writes=[m], sem_buf=m)
            x = xt[0]
            P.dma("sp", x[:, :], xo[tt * 128:(tt + 1) * 128, :], writes=[x], sem_buf=x)
            xx = x2[tt % 2]
            for cg in range(4):
                pb = banks[nb % 4]
                nb += 1
                for k in range(16):
                    P.op("pe", lambda e, k=k, cg=cg, pb=pb, mv=mv, j=j: e.matmul(
                        pb[:, :], lhsT=mv[:, k, j * 128:(j + 1) * 128], rhs=wov[:, k, cg * 512:(cg + 1) * 512],
                        start=(k == 0), stop=(k == 15)), reads=[m, wo], writes=[pb])
                P.op("dve", lambda e, cg=cg, pb=pb, x=x, xx=xx: e.tensor_tensor(out=xx[:, cg * 512:(cg + 1) * 512], in0=pb[:, :],
                                                                              in1=x[:, cg * 512:(cg + 1) * 512], op=ALU.add),
                     reads=[pb, x], writes=[xx])
            P.dma("act", x2d[tt * 128:(tt + 1) * 128, :], xx[:, :], reads=[xx], sem_buf=xx)
            xnb = xn[tt % 2]
            norm_rows(xx[:, :], xx, gb, xnb, ss[tt % 2], rs[tt % 2], sq)
            P.dma("act", x2n[tt * 128:(tt + 1) * 128, :], xnb[:, :], reads=[xnb], sem_buf=xnb)
            xT = xnT[tt % 2]
            xTv = xT.ap.rearrange("p (k t) -> p k t", k=16)
            transpose_rows(xnb, lambda hlf, xTv=xTv: xTv[:, hlf * 8:(hlf + 1) * 8, :], xT, pbanks[0])
            for k in range(16):
                P.op("pe", lambda e, k=k, xTv=xTv: e.matmul(bL[:, 0:36], lhsT=xTv[:, k, :], rhs=wrv[:, k, :], start=(k == 0), stop=(k == 15)),
                     reads=[xT, wr], writes=[bL])
            P.op("dve", lambda e: e.tensor_tensor(out=lg[:, :], in0=bL[:, 0:36], in1=br[:, :], op=ALU.add), reads=[bL, br], writes=[lg])
            P.op("dve", lambda e: e.tensor_reduce(out=gmax[:, :], in_=lg[:, 0:4], axis=AX.X, op=ALU.max), reads=[lg], writes=[gmax])
            P.op("dve", lambda e: e.tensor_scalar(out=ngmax[:, :], in0=gmax[:, :], scalar1=-1.0, scalar2=None, op0=ALU.mult),
                 reads=[gmax], writes=[ngmax])
            P.op("act", lambda e: e.activation(out=gex[:, :], in_=lg[:, 0:4], func=AF.Exp, bias=ngmax[:, :], accum_out=gsum[:, :]),
                 reads=[lg, ngmax], writes=[gex, gsum])
            P.op("dve", lambda e: e.reciprocal(out=gw[:, :], in_=gsum[:, :]), reads=[gsum], writes=[gw])
            P.op("dve", lambda e: e.tensor_scalar(out=gmk[:, :], in0=lg[:, 0:4], scalar1=gmax[:, 0:1], scalar2=-1.0,
                                                  op0=ALU.is_ge, op1=ALU.add), reads=[lg, gmax], writes=[gmk])
            P.op("dve", lambda e: e.scalar_tensor_tensor(
                out=lem.ap.rearrange("p (g k) -> p g k", g=4), in0=gmk.ap.unsqueeze(2).to_broadcast([128, 4, 8]), scalar=BIG,
                in1=lg.ap[:, 4:36].rearrange("p (g k) -> p g k", g=4), op0=ALU.mult, op1=ALU.add), reads=[gmk, lg], writes=[lem])
            P.op("dve", lambda e: e.max(out=m8[:, :], in_=lem[:, :]), reads=[lem], writes=[m8])
            P.op("dve", lambda e, tt=tt: e.tensor_scalar(out=s1v[:, tt, :], in0=lem[:, :], scalar1=m8[:, 0:1], scalar2=None, op0=ALU.is_ge),
                 reads=[lem, m8], writes=[sel1])
            P.op("dve", lambda e, tt=tt: e.tensor_scalar(out=s2v[:, tt, :], in0=lem[:, :], scalar1=m8[:, 1:2], scalar2=None, op0=ALU.is_ge),
                 reads=[lem, m8], writes=[sel2])
            P.op("dve", lambda e, tt=tt: e.tensor_tensor(out=s2v[:, tt, :], in0=s2v[:, tt, :], in1=s1v[:, tt, :], op=ALU.subtract),
                 reads=[sel1, sel2], writes=[sel2])
            P.op("dve", lambda e, tt=tt: e.tensor_tensor(out=sav[:, tt, :], in0=s1v[:, tt, :], in1=s2v[:, tt, :], op=ALU.add),
                 reads=[sel1, sel2], writes=[selall])
            P.op("dve", lambda e: e.tensor_tensor(out=dd[:, :], in0=m8[:, 1:2], in1=m8[:, 0:1], op=ALU.subtract), reads=[m8], writes=[dd])
            P.op("act", lambda e: e.activation(out=dd[:, :], in_=dd[:, :], func=AF.Exp), reads=[dd], writes=[dd])
            P.op("dve", lambda e: e.tensor_scalar(out=dd[:, :], in0=dd[:, :], scalar1=1.0, scalar2=None, op0=ALU.add), reads=[dd], writes=[dd])
            P.op("dve", lambda e: e.reciprocal(out=dd[:, :], in_=dd[:, :]), reads=[dd], writes=[dd])
            P.op("dve", lambda e, tt=tt: e.tensor_tensor(out=wv_[:, tt, 0:1], in0=dd[:, :], in1=gw[:, :], op=ALU.mult), reads=[dd, gw], writes=[wgt])
            P.op("dve", lambda e, tt=tt: e.tensor_tensor(out=wv_[:, tt, 1:2], in0=gw[:, :], in1=wv_[:, tt, 0:1], op=ALU.subtract),
                 reads=[gw, wgt], writes=[wgt])
            for t2 in range(tt + 1):
                P.op("pe", lambda e, t2=t2, tt=tt: e.matmul(bC[:, 0:32], lhsT=(trib[:, :] if t2 == tt else onesb[:, :]), rhs=sav[:, t2, :],
                                                           start=(t2 == 0), stop=(t2 == tt)), reads=[trib, onesb, selall], writes=[bC])
            P.op("act", lambda e, tt=tt: e.activation(out=posv[:, tt, :], in_=bC[:, 0:32], func=AF.Copy), reads=[bC], writes=[posall])
        cnt = P.alloc(32, F32, "cnt")
        nt_ = P.alloc(32, F32, "nt_")
        fr = P.alloc(32, F32, "fr")
        cum = P.alloc(32, F32, "cum")
        base = P.alloc(32, F32, "base")
        one32 = P.alloc(32, F32, "one32")
        z1 = P.alloc(1, F32, "z1")
        for t2 in range(32):
            P.op("pe", lambda e, t2=t2: e.matmul(bC[:, 0:32], lhsT=onesb[:, :], rhs=sav[:, t2, :], start=(t2 == 0), stop=(t2 == 31)),
                 reads=[onesb, selall], writes=[bC])
        th = P.alloc(NT_E * 32, F32, "th")
        P.dma("sp", th[:, :], thr96[:, :], writes=[th], sem_buf=th)
        cmp_ = P.alloc(NT_E * 32, F32, "cmp")
        P.op("dve", lambda e: e.tensor_scalar(out=cnt[:, :], in0=bC[:, 0:32], scalar1=1.0 / TSL, scalar2=None, op0=ALU.mult),
             reads=[bC], writes=[cnt])
        P.op("dve", lambda e: e.tensor_tensor(out=cmp_.ap.rearrange("p (t e) -> p t e", t=NT_E),
                                              in0=cnt.ap.unsqueeze(1).to_broadcast([128, NT_E, 32]),
                                              in1=th.ap.rearrange("p (t e) -> p t e", t=NT_E), op=ALU.is_gt),
             reads=[th, cnt], writes=[cmp_])
        P.op("dve", lambda e: e.tensor_reduce(out=nt_[:, :], in_=cmp_.ap.rearrange("p (t e) -> p e t", t=NT_E), axis=AX.X, op=ALU.add),
             reads=[cmp_], writes=[nt_])
        P.op("dve", lambda e: e.memset(one32[:, :], 1.0), writes=[one32])
        P.op("dve", lambda e: e.memset(z1[:, :], 0.0), writes=[z1])
        P.op("dve", lambda e: e.tensor_tensor_scan(out=cum[:, :], data0=one32[:, :], data1=nt_[:, :], initial=z1[:, 0:1],
                                                   op0=ALU.mult, op1=ALU.add), reads=[one32, nt_, z1], writes=[cum])
        P.op("dve", lambda e: e.tensor_tensor(out=base[:, :], in0=cum[:, :], in1=nt_[:, :], op=ALU.subtract), reads=[cum, nt_], writes=[base])
        P.op("dve", lambda e: e.tensor_scalar(out=base[:, :], in0=base[:, :], scalar1=float(TSL), scalar2=None, op0=ALU.mult), reads=[base], writes=[base])
        P.op("dve", lambda e: e.tensor_tensor(out=cmp_.ap.rearrange("p (t e) -> p t e", t=NT_E),
                                              in0=th.ap.rearrange("p (t e) -> p t e", t=NT_E),
                                              in1=cum.ap.unsqueeze(1).to_broadcast([128, NT_E, 32]), op=ALU.is_ge),
             reads=[th, cum], writes=[cmp_])
        P.op("dve", lambda e: e.tensor_reduce(out=te[:, :], in_=cmp_.ap.rearrange("p (t e) -> p t e", t=NT_E), axis=AX.X, op=ALU.add),
             reads=[cmp_], writes=[te])
        P.op("dve", lambda e: e.tensor_scalar(out=te[:, :], in0=te[:, :], scalar1=31.0, scalar2=None, op0=ALU.min), reads=[te], writes=[te])
        slotf = P.alloc(1, F32, "slotf")
        sloti = [P.alloc(1, I32, f"sloti{i}") for i in range(2)]
        rc = [P.alloc(4, F32, f"rc{i}") for i in range(2)]
        tmp32 = P.alloc(32, F32, "tmp32")
        init_tok = P.dma("sp", recs[:, :], recinit[:, :], sem_buf=th)
        it = 0
        for tt in range(32):
            for kk, sv in ((0, s1v), (1, s2v)):
                P.op("dve", lambda e, tt=tt: e.tensor_tensor(out=tmp32[:, :], in0=posv[:, tt, :], in1=base[:, :], op=ALU.add),
                     reads=[posall, base], writes=[tmp32])
                P.op("dve", lambda e, tt=tt, sv=sv: e.tensor_tensor(out=tmp32[:, :], in0=tmp32[:, :], in1=sv[:, tt, :], op=ALU.mult),
                     reads=[tmp32, sel1, sel2], writes=[tmp32])
                P.op("dve", lambda e: e.tensor_reduce(out=slotf[:, :], in_=tmp32[:, :], axis=AX.X, op=ALU.add), reads=[tmp32], writes=[slotf])
                si = sloti[it % 2]
                r_ = rc[it % 2]
                P.op("dve", lambda e, si=si: e.tensor_copy(out=si[:, :], in_=slotf[:, :]), reads=[slotf], writes=[si])
                P.op("dve", lambda e, r_=r_, tt=tt: e.tensor_copy(out=r_[:, 0:1], in_=iob[:, tt:tt + 1]), reads=[iob], writes=[r_])
                P.op("dve", lambda e, r_=r_, tt=tt, kk=kk: e.tensor_scalar(out=r_[:, 1:2], in0=iob[:, tt:tt + 1], scalar1=float(kk * TOWN),
                                                                        scalar2=None, op0=ALU.add), reads=[iob], writes=[r_])
                P.op("dve", lambda e, r_=r_, tt=tt, kk=kk: e.tensor_copy(out=r_[:, 2:3], in_=wv_[:, tt, kk:kk + 1]), reads=[wgt], writes=[r_])
                P.op("dve", lambda e, r_=r_: e.memset(r_[:, 3:4], 0.0), writes=[r_])
                P.raw("pool", lambda e, si=si, r_=r_: e.indirect_dma_start(
                    out=recs[:, :], out_offset=bass.IndirectOffsetOnAxis(ap=si[:, :], axis=0), in_=r_[:, :], in_offset=None,
                    ), reads=[si, r_], extra=[init_tok], sem_buf=si)
                it += 1
        P.barrier()
        return te

    tei = None
    if stage >= 6:
        tei = phase_wout()


    def phase_moe(te_unused):
        P.reset_arena(base_off)
        ewg_f = ewgb.rearrange("e (p h j) n -> (e p h) (j n)", h=2, j=8)
        ewu_f = ewub.rearrange("e (p h j) n -> (e p h) (j n)", h=2, j=8)
        ewd_f = ewdb.rearrange("e (p h j) n -> (e p h) (j n)", h=2, j=2)
        ixi = [P.alloc(2, I32, f"ixi{i}") for i in range(2)]
        e2k = P.alloc(2, F32, "e2k")
        e2f = P.alloc(2, F32, "e2f")
        iob2 = P.alloc(2, F32, "iob2")
        P.op("dve", lambda e: e.tensor_scalar(out=iob2[:, 0:1], in0=iob[:, 0:1], scalar1=2.0, scalar2=None, op0=ALU.mult),
             reads=[iob], writes=[iob2])
        P.op("dve", lambda e: e.tensor_scalar(out=iob2[:, 1:2], in0=iob[:, 0:1], scalar1=2.0, scalar2=1.0, op0=ALU.mult, op1=ALU.add),
             reads=[iob], writes=[iob2])
        wun2 = [[P.alloc(4096, BF16, f"wun{b}_{i}") for i in range(6)] for b in range(2)]
        rc = [P.alloc(4, F32, f"rcm{i}") for i in range(3)]
        gi = [P.alloc(1, I32, f"gi{i}") for i in range(3)]
        si = [P.alloc(1, I32, f"si{i}") for i in range(3)]
        xg = [P.alloc(D, BF16, f"xg{i}") for i in range(3)]
        xgT = [P.alloc(16 * 128, BF16, f"xgT{i}") for i in range(2)]
        hs = P.alloc(HID, F32, "hs")
        ab = [P.alloc(HID, BF16, f"ab{i}") for i in range(2)]
        aT = [P.alloc(4 * 128, BF16, f"aT{i}") for i in range(2)]
        yo = [P.alloc(D, F32, f"yom{i}") for i in range(1)]
        bGs, bUs = [P.bank(0), P.bank(1)], [P.bank(2), P.bank(3)]
        bY = [P.bank(4), P.bank(5)]
        pbk = (P.bank(6, BF16), P.bank(7, BF16))
        NSUB = TSL // 128

        def wviews(t):
            wun = wun2[t % 2]
            gvh = [wun[0].ap.rearrange("p (k n) -> p k n", k=8), wun[1].ap.rearrange("p (k n) -> p k n", k=8)]
            uvh = [wun[2].ap.rearrange("p (k n) -> p k n", k=8), wun[3].ap.rearrange("p (k n) -> p k n", k=8)]
            dvh = [wun[4].ap.rearrange("p (k n) -> p k n", k=2), wun[5].ap.rearrange("p (k n) -> p k n", k=2)]
            return wun, gvh, uvh, dvh

        def emit_gathers(t):
            ix_ = ixi[t % 2]
            P.op("dve", lambda e: e.tensor_scalar(out=e2k[:, 0:1], in0=te[:, t:t + 1], scalar1=256.0, scalar2=None, op0=ALU.mult),
                 reads=[te], writes=[e2k])
            P.op("dve", lambda e: e.tensor_scalar(out=e2f[:, 0:2], in0=iob2[:, 0:2], scalar1=e2k[:, 0:1], scalar2=None, op0=ALU.add),
                 reads=[iob2, e2k], writes=[e2f])
            P.op("dve", lambda e: e.tensor_copy(out=ix_[:, 0:2], in_=e2f[:, 0:2]), reads=[e2f], writes=[ix_])

            for u in range(6):
                src_flat = (ewg_f, ewu_f, ewd_f)[u // 2]
                hcol = (u % 2) * 4096
                st = wun2[t % 2][u]
                P.raw("pool", lambda e: e.indirect_dma_start(
                    out=st[:, :], out_offset=None, in_=src_flat,
                    in_offset=bass.IndirectOffsetOnAxis(ap=ix_[:, (u % 2):(u % 2) + 1], axis=0)),
                    reads=[ix_], writes=[st], sem_buf=st)

        def emit_casts(t):
            for u in range(6):
                st = stg[u]
                wb = wun[u]
                if u % 2 == 0:
                    P.op("act", lambda e: e.activation(out=wb[:, :], in_=st[:, :], func=AF.Copy), reads=[st], writes=[wb])
                else:
                    P.op("dve", lambda e: e.tensor_copy(out=wb[:, :], in_=st[:, :]), reads=[st], writes=[wb])


        def emit_xg(q):
            r_ = rc[q % 3]
            P.dma("sp", r_[:, :], recs[q * 128:(q + 1) * 128, :], writes=[r_], sem_buf=r_)
            gi_, si_ = gi[q % 3], si[q % 3]
            P.op("dve", lambda e: e.tensor_copy(out=gi_[:, :], in_=r_[:, 0:1]), reads=[r_], writes=[gi_])
            P.op("dve", lambda e: e.tensor_copy(out=si_[:, :], in_=r_[:, 1:2]), reads=[r_], writes=[si_])
            x_ = xg[q % 3]
            P.raw("pool", lambda e: e.indirect_dma_start(
                out=x_[:, :], out_offset=None, in_=x2n[:, :], in_offset=bass.IndirectOffsetOnAxis(ap=gi_[:, :], axis=0),
                ), reads=[gi_], writes=[x_], sem_buf=x_)

        def emit_A(q):
            x_ = xg[q % 3]
            xT = xgT[q % 2]
            xTv = xT.ap.rearrange("p (k t) -> p k t", k=16)
            bG, bU = bGs[q % 2], bUs[q % 2]
            wun, gvh, uvh, dvh = wviews(q // NSUB)
            transpose_rows(x_, lambda hlf: xTv[:, hlf * 8:(hlf + 1) * 8, :], xT, pbk, strided=True)
            for k in range(16):
                P.op("pe", lambda e: e.matmul(bG[:, :], lhsT=xTv[:, k, :], rhs=gvh[k // 8][:, k % 8, :], start=(k == 0), stop=(k == 15)),
                     reads=[xT, wun[k // 8]], writes=[bG])
            for k in range(16):
                P.op("pe", lambda e: e.matmul(bU[:, :], lhsT=xTv[:, k, :], rhs=uvh[k // 8][:, k % 8, :], start=(k == 0), stop=(k == 15)),
                     reads=[xT, wun[2 + k // 8]], writes=[bU])

        def emit_B(q):
            r_ = rc[q % 3]
            si_ = si[q % 3]
            bG, bU = bGs[q % 2], bUs[q % 2]
            wun, gvh, uvh, dvh = wviews(q // NSUB)
            P.op("act", lambda e: e.activation(out=hs[:, :], in_=bG[:, :], func=AF.Silu), reads=[bG], writes=[hs])
            a_ = ab[q % 2]
            P.op("dve", lambda e: e.tensor_tensor(out=a_[:, :], in0=hs[:, :], in1=bU[:, :], op=ALU.mult), reads=[hs, bU], writes=[a_])
            aT_ = aT[q % 2]
            pb = pbk[0]
            for k in range(4):
                P.op("pe", lambda e: e.transpose(out=pb[:, k * 128:(k + 1) * 128], in_=a_.ap.rearrange("s (p j) -> s j p", j=4)[:, k, :],
                                                 identity=idb[:, :]), reads=[a_, idb], writes=[pb])
            P.op("act", lambda e: e.activation(out=aT_[:, :], in_=pb[:, 0:512], func=AF.Copy), reads=[pb], writes=[aT_])
            y_ = yo[0]
            for cg in range(4):
                by = bY[cg % 2]
                for k in range(4):
                    P.op("pe", lambda e: e.matmul(by[:, :], lhsT=aT_[:, k * 128:(k + 1) * 128], rhs=dvh[k // 2][:, k % 2, cg * 512:(cg + 1) * 512],
                                                  start=(k == 0), stop=(k == 3)), reads=[aT_, wun[4 + k // 2]], writes=[by])
                if cg % 2 == 0:
                    P.op("act", lambda e: e.activation(out=y_[:, cg * 512:(cg + 1) * 512], in_=by[:, :], func=AF.Copy, scale=r_[:, 2:3]),
                         reads=[by, r_], writes=[y_])
                else:
                    P.op("dve", lambda e: e.tensor_scalar(out=y_[:, cg * 512:(cg + 1) * 512], in0=by[:, :], scalar1=r_[:, 2:3], scalar2=None,
                                                          op0=ALU.mult), reads=[by, r_], writes=[y_])
            P.raw("pool", lambda e: e.indirect_dma_start(
                out=ybuf[:, :], out_offset=bass.IndirectOffsetOnAxis(ap=si_[:, :], axis=0), in_=y_[:, :], in_offset=None,
                ), reads=[si_, y_], sem_buf=y_)

        def emit_casts_part(t, units):
            for u in units:
                st = stg[u]
                wb = wun[u]
                if u % 2 == 0:
                    P.op("act", lambda e: e.activation(out=wb[:, :], in_=st[:, :], func=AF.Copy), reads=[st], writes=[wb])
                else:
                    P.op("dve", lambda e: e.tensor_copy(out=wb[:, :], in_=st[:, :]), reads=[st], writes=[wb])

        NQ = NT_E * NSUB
        emit_gathers(0)
        emit_gathers(1)
        emit_xg(0)
        emit_xg(1)
        emit_A(0)
        for q in range(NQ):
            t, sub = q // NSUB, q % NSUB
            if q + 2 < NQ:
                emit_xg(q + 2)
            if q + 1 < NQ:
                emit_A(q + 1)
            emit_B(q)
            if sub == NSUB - 1 and t + 2 < NT_E:
                emit_gathers(t + 2)
        P.barrier()
        P.release_dma_sems(rc + xg + yo + wun2[0] + wun2[1])

    def phase_final():
        P.reset_arena(base_off)
        gb = P.alloc(D, F32, "gbz")
        P.dma("sp", gb[:, :], gz_b[:, :], writes=[gb], sem_buf=gb)
        xa_ = [P.alloc(D, F32, f"fa{i}") for i in range(2)]
        y1 = [P.alloc(D, F32, f"fb{i}") for i in range(2)]
        y2 = [P.alloc(D, F32, f"fc{i}") for i in range(2)]
        ob = [P.alloc(D, F32, f"fo{i}") for i in range(2)]
        sq = P.alloc(D, F32, "fsq")
        ss = [P.alloc(1, F32) for i in range(2)]
        rs = [P.alloc(1, F32) for i in range(2)]
        for tt in range(32):
            a, b, c, o = xa_[tt % 2], y1[tt % 2], y2[tt % 2], ob[tt % 2]
            rows = slice(tt * 128, (tt + 1) * 128)
            P.dma("sp", a[:, :], x2d[rows, :], writes=[a], sem_buf=a)
            P.dma("sp", b[:, :], ybuf[rows, :], writes=[b], sem_buf=b)
            P.dma("sp", c[:, :], ybuf[TOWN + tt * 128:TOWN + (tt + 1) * 128, :], writes=[c], sem_buf=c)
            P.op("pool", lambda e: e.tensor_tensor(out=b[:, :], in0=b[:, :], in1=c[:, :], op=ALU.add), reads=[b, c], writes=[b])
            P.op("dve", lambda e: e.tensor_tensor(out=a[:, :], in0=a[:, :], in1=b[:, :], op=ALU.add), reads=[a, b], writes=[a])
            s_, r_ = ss[tt % 2], rs[tt % 2]
            P.op("act", lambda e: e.activation(out=sq[:, :], in_=a[:, :], func=AF.Square, accum_out=s_[:, :]), reads=[a], writes=[sq, s_])
            P.op("act", lambda e: e.activation(out=r_[:, :], in_=s_[:, :], func=AF.Sqrt, scale=1.0 / D, bias=epsb[:, :]),
                 reads=[s_, epsb], writes=[r_])
            P.op("dve", lambda e: e.reciprocal(out=r_[:, :], in_=r_[:, :]), reads=[r_], writes=[r_])
            P.op("dve", lambda e: e.scalar_tensor_tensor(out=o[:, :], in0=a[:, :], scalar=r_[:, 0:1], in1=gb[:, :], op0=ALU.mult, op1=ALU.mult),
                 reads=[a, r_, gb], writes=[o])
            P.dma("act", out[rows, :], o[:, :], reads=[o], sem_buf=o)
        P.barrier()

    if stage >= 7:
        phase_moe(tei)
    if stage >= 8:
        phase_final()
    P.barrier()
    P.emit()
    return nc


def core_inputs(inp, core):
    b, p = core // 2, core % 2
    l = 0
    f = np.float32
    x = inp["x"][b]
    xo = x.reshape(16, 2, 256, D)[:, p].reshape(TOWN, D)
    d = {}
    d["xa"] = np.ascontiguousarray(x); d["xo"] = np.ascontiguousarray(xo)
    d["w_in"] = inp["w_in"][l]
    d["ga_b"] = np.ascontiguousarray(np.broadcast_to(inp["norm_attn_g"][l][None, :], (128, D)))
    d["gf_b"] = np.ascontiguousarray(np.broadcast_to(inp["norm_ffn_g"][l][None, :], (128, D)))
    d["gz_b"] = np.ascontiguousarray(np.broadcast_to(inp["norm_final_g"][None, :], (128, D)))
    d["ident"] = np.eye(128, dtype=f)
    cw = inp["conv_w"][l].reshape(4, 8, 128)
    d["convw"] = np.ascontiguousarray(cw.transpose(2, 1, 0).reshape(128, 32))
    lv = np.stack([inp["conv_b"][l], inp["lru_ba"][l], inp["lru_bx"][l], inp["lru_lambda"][l]], 0).reshape(4, 8, 128)
    d["lruv"] = np.ascontiguousarray(lv.transpose(2, 1, 0).reshape(128, 32))
    d["lru_wa"] = inp["lru_wa"][l]; d["lru_wx"] = inp["lru_wx"][l]
    d["par"] = np.ascontiguousarray(np.broadcast_to(np.array([[1.0 - p, float(p)]], f), (128, 2)))
    LT = np.zeros((NH, 36, 64, 128), f); RC = np.zeros((NH, 4, 16, 256), f)
    for h in range(NH):
        sl = 2.0 ** (-(h + 1))
        for c in range(64):
            LT[h, c // 2, c, :] = 1.0
            LT[h, 35, c, :] = sl * 128 * c
        LT[h, 32] = 1.0; LT[h, 33] = 1.0
        LT[h, 34] = sl * np.arange(128, dtype=f)[None, :]
        RC[h, 0] = -sl * np.arange(256, dtype=f)[None, :]
        for s in range(16):
            RC[h, 1, s, :] = -sl * 256 * (2 * s + p)
        RC[h, 2] = 1.0; RC[h, 3] = 1.0
    d["LTt"] = LT.reshape(NH, 36, 64 * 128); d["RCt"] = RC.reshape(NH, 4, 16 * 256)
    NM = np.zeros((128, 4, 256), f)
    ki = np.arange(128)[:, None]; qi = np.arange(256)[None, :]
    for r in range(4):
        blk, ch = r // 2, r % 2
        if blk == p:
            NM[:, r, :] = np.where(ch * 128 + ki <= qi, 0.0, -BIG)
        elif blk > p:
            NM[:, r, :] = -BIG
        else:
            NM[:, r, :] = 0.0
    NM2 = np.zeros((128, 8, 512), f)
    NM2[:, 0:4, 0:256] = NM
    NM2[:, 4:8, 0:256] = -BIG
    NM2[:, 0:4, 256:512] = 0.0
    NM2[:, 4:8, 256:512] = NM
    d["NM2t"] = NM2.reshape(128, 8 * 512)
    n = np.arange(32)[None, :]; s = np.arange(16)[:, None]
    past = (n < 2 * s + p)
    d["PBt"] = np.ascontiguousarray(np.broadcast_to(np.where(past, 0.0, -BIG).astype(f).reshape(1, 512), (128, 512)))
    d["VMt"] = np.ascontiguousarray(np.broadcast_to((past * BIG).astype(f).reshape(1, 512), (128, 512)))
    d["proj_rec"] = inp["proj_rec"][l]; d["proj_attn"] = inp["proj_attn"][l]; d["w_out"] = inp["w_out"][l]
    d["w_r"] = np.ascontiguousarray(np.concatenate([inp["router_group_w"][l], inp["router_expert_w"][l]], 1))
    d["b_r"] = np.ascontiguousarray(np.broadcast_to(np.concatenate([inp["router_group_b"][l], inp["router_expert_b"][l]])[None, :], (128, 36)))
    d["ewg"] = inp["expert_w_gate"][l]; d["ewu"] = inp["expert_w_up"][l]; d["ewd"] = inp["expert_w_down"][l]
    d["tri"] = np.triu(np.ones((128, 128), f), 1)
    ri = np.zeros((NSLOT, 4), f); ri[:, 0] = TOWN; ri[:, 1] = 2 * TOWN
    d["recinit"] = ri
    d["thr96"] = np.ascontiguousarray(np.broadcast_to(np.repeat(np.arange(NT_E, dtype=f), 32)[None, :], (128, NT_E * 32)))
    d["iot"] = (np.arange(32)[None, :] * 128 + np.arange(128)[:, None]).astype(f)
    return d


_NC_CACHE = {}


def kernel(**inputs):
    inp = {k: np.asarray(v) for k, v in inputs.items()}
    if "nc" not in _NC_CACHE:
        _NC_CACHE["nc"] = build()
    nc = _NC_CACHE["nc"]
    in_maps = [core_inputs(inp, c) for c in range(8)]
    res = run_bass_kernel_spmd(nc, in_maps, core_ids=list(range(8)))
    full = np.zeros((4, SEQ, D), np.float32)
    for c in range(8):
        b, p = c // 2, c % 2
        o = np.asarray(res.results[c]["out"]).reshape(16, 256, D)
        full[b].reshape(16, 2, 256, D)[:, p] = o
    return full
```
